# Optimizing a Trainium2 kernel written in Bass

```python
import jax
import jax.numpy as jnp
from jax import lax
import numpy as np

D_MODEL = 2048
BATCH = 1
SEQ = 8192
DEPTH = 4

f32 = jnp.float32
GRID_W = 64
CTX_LEN = 256
N_MIXERS = 3
N_RET_LAYERS = (DEPTH + 2) // 3
N_MLSTM_LAYERS = (DEPTH + 1) // 3
N_ATTN_LAYERS = DEPTH // 3
CHUNK = 128
NORM_EPS = 1e-6
ADA_INIT = 0.5

RET_HEADS = 8
RET_DK = D_MODEL // RET_HEADS
RET_DV = 2 * RET_DK
RET_IN = 2 * RET_HEADS * RET_DK + 2 * RET_HEADS * RET_DV
RET_ROPE_BASE = 10000.0

MLSTM_HEADS = 8
MLSTM_DK = D_MODEL // (2 * MLSTM_HEADS)
MLSTM_DV = D_MODEL // MLSTM_HEADS
MLSTM_QK = MLSTM_HEADS * MLSTM_DK
MLSTM_V = MLSTM_HEADS * MLSTM_DV
MLSTM_IN = 2 * MLSTM_QK + 2 * MLSTM_V + 4 * MLSTM_HEADS
MLSTM_CONV = 3
GATE_SOFTCAP = 15.0

ATTN_HEADS = 16
ATTN_KV_HEADS = 8
ATTN_HD = D_MODEL // ATTN_HEADS
ATTN_GROUPS = ATTN_HEADS // ATTN_KV_HEADS
ATTN_IN = ATTN_HEADS * ATTN_HD + 2 * ATTN_KV_HEADS * ATTN_HD
Q_BLOCK = 128
ROPE_BASE = 10000.0

MOE_GROUPS = 4
MOE_PER_GROUP = 8
MOE_EXPERTS = MOE_GROUPS * MOE_PER_GROUP
MOE_TOPK = 2
MOE_FF = 3 * D_MODEL // 8
MOE_BLOCK = 128

kernel_name = 'hybrid_ret_mlstm_gqa_hmoe_dit'


def rmsnorm(x, g):
    x32 = x.astype(f32)
    y = x32 * lax.rsqrt(jnp.mean(x32 * x32, axis=-1, keepdims=True) + NORM_EPS)
    return (y * g.astype(f32)).astype(x.dtype)


def modulate(h, shift, scale):
    return h * (1.0 + scale) + shift


def flip_parts(y, n_ctx):
    return jnp.concatenate([jnp.flip(y[:, :n_ctx], axis=1), jnp.flip(y[:, n_ctx:], axis=1)], axis=1)


def split_ctx_lat(y, n_ctx, need_ctx):
    if need_ctx:
        return y[:, :n_ctx], y[:, n_ctx:]
    return None, y


def rope_tables(pos, dim, base):
    inv = jnp.power(base, -jnp.arange(0, dim, 2, dtype=f32) / dim)
    ang = pos.astype(f32)[:, None] * inv[None, :]
    return jnp.cos(ang), jnp.sin(ang)


def apply_rope(x, cos, sin):
    x32 = x.astype(f32)
    x1, x2 = jnp.split(x32, 2, axis=-1)
    c, s = cos[None, :, None, :], sin[None, :, None, :]
    return jnp.concatenate([x1 * c - x2 * s, x1 * s + x2 * c], axis=-1).astype(x.dtype)


def axial_rope_tables(n_ctx, n_lat):
    rows = n_lat // GRID_W
    row = jnp.repeat(jnp.arange(rows, dtype=jnp.int32), GRID_W)
    col = jnp.tile(jnp.arange(GRID_W, dtype=jnp.int32), rows)
    half = ATTN_HD // 2
    cr, sr = rope_tables(row, half, ROPE_BASE)
    cc, sc = rope_tables(col, half, ROPE_BASE)
    one = jnp.ones((n_ctx, half // 2), f32)
    zero = jnp.zeros((n_ctx, half // 2), f32)
    cat = lambda fill, t: jnp.concatenate([fill, t], axis=0)
    return cat(one, cr), cat(zero, sr), cat(one, cc), cat(zero, sc)


def apply_axial_rope(x, cos_r, sin_r, cos_c, sin_c):
    half = x.shape[-1] // 2
    return jnp.concatenate([apply_rope(x[..., :half], cos_r, sin_r),
                            apply_rope(x[..., half:], cos_c, sin_c)], axis=-1)


def dwconv(x, w):
    k = w.shape[0]
    return lax.conv_general_dilated(x, w[:, None, :], window_strides=(1,), padding=[(k // 2, k // 2)],
                                    dimension_numbers=('NWC', 'WIO', 'NWC'),
                                    feature_group_count=x.shape[-1])


def to_chunks(t):
    bsz, L, H, d = t.shape
    return t.reshape(bsz, L // CHUNK, CHUNK, H, d).transpose(1, 0, 3, 2, 4)


def from_chunks(t):
    nc, bsz, H, C, d = t.shape
    return t.transpose(1, 0, 3, 2, 4).reshape(bsz, nc * C, H, d)


def gate_chunks(t):
    bsz, L, H = t.shape
    return t.reshape(bsz, L // CHUNK, CHUNK, H).transpose(1, 0, 3, 2)


def retention_scan(q, k, v, log_gamma):
    bsz, L, H, DK = q.shape
    DV = v.shape[-1]
    pos = jnp.arange(CHUNK, dtype=f32)
    diff = pos[:, None] - pos[None, :]
    lower = diff >= 0
    lg = log_gamma[:, None, None]
    decay = jnp.where(lower[None], jnp.exp(jnp.where(lower, diff, 0.0)[None] * lg), 0.0)
    q_dec = jnp.exp((pos[None, :] + 1.0) * log_gamma[:, None])[..., None]
    k_dec = jnp.exp((CHUNK - 1.0 - pos[None, :]) * log_gamma[:, None])[..., None]
    c_dec = jnp.exp(CHUNK * log_gamma)[:, None, None]

    def step(S, xs):
        qb, kb, vb = xs
        scores = jnp.einsum('bhid,bhjd->bhij', qb, kb) * decay
        inner = jnp.einsum('bhij,bhjv->bhiv', scores, vb)
        cross = jnp.einsum('bhid,bhdv->bhiv', qb * q_dec, S)
        S_new = c_dec * S + jnp.einsum('bhjd,bhjv->bhdv', kb * k_dec, vb)
        return S_new, inner + cross

    S0 = jnp.zeros((bsz, H, DK, DV), f32)
    _, out = lax.scan(step, S0, (to_chunks(q), to_chunks(k), to_chunks(v)))
    return from_chunks(out)


def mlstm_scan(q, k, v, i_pre, log_f):
    bsz, L, H, DK = q.shape
    DV = v.shape[-1]
    lower = jnp.tril(jnp.ones((CHUNK, CHUNK), bool))

    def step(carry, xs):
        C_prev, n_prev, m_prev = carry
        qb, kb, vb, ib, fb = xs
        b = jnp.cumsum(fb, axis=-1)
        d_log = jnp.where(lower, b[..., :, None] - b[..., None, :] + ib[..., None, :], -jnp.inf)
        m_t = jnp.maximum(b + m_prev[..., None], d_log.max(-1))
        w = jnp.exp(d_log - m_t[..., None])
        s = jnp.einsum('bhid,bhjd->bhij', qb, kb) * w
        inter = jnp.exp(b + m_prev[..., None] - m_t)
        num = jnp.einsum('bhij,bhjv->bhiv', s, vb) + inter[..., None] * jnp.einsum('bhid,bhdv->bhiv', qb, C_prev)
        den = s.sum(-1) + inter * jnp.einsum('bhid,bhd->bhi', qb, n_prev)
        h = num / jnp.maximum(jnp.abs(den), jnp.exp(-m_t))[..., None]
        b_last = b[..., -1:]
        g = b_last - b + ib
        m_new = jnp.maximum(b_last[..., 0] + m_prev, g.max(-1))
        wk = jnp.exp(g - m_new[..., None])[..., None] * kb
        dec = jnp.exp(b_last[..., 0] + m_prev - m_new)
        C_new = dec[..., None, None] * C_prev + jnp.einsum('bhjd,bhjv->bhdv', wk, vb)
        n_new = dec[..., None] * n_prev + wk.sum(-2)
        return (C_new, n_new, m_new), h

    init = (jnp.zeros((bsz, H, DK, DV), f32), jnp.zeros((bsz, H, DK), f32), jnp.zeros((bsz, H), f32))
    _, out = lax.scan(step, init, (to_chunks(q), to_chunks(k), to_chunks(v), gate_chunks(i_pre), gate_chunks(log_f)))
    return from_chunks(out)


def retention_mixer(h_ctx, h_lat, w_in, logit_gamma, gn_w, gn_b, w_out, need_ctx):
    n_ctx = h_ctx.shape[1]
    h = jnp.concatenate([h_ctx, h_lat], axis=1)
    bsz, L, _ = h.shape
    qk_w, v_w = RET_HEADS * RET_DK, RET_HEADS * RET_DV
    q, k, v, g = jnp.split(h @ w_in, [qk_w, 2 * qk_w, 2 * qk_w + v_w], axis=-1)
    q = q.reshape(bsz, L, RET_HEADS, RET_DK) * (RET_DK ** -0.5)
    k = k.reshape(bsz, L, RET_HEADS, RET_DK)
    v = v.reshape(bsz, L, RET_HEADS, RET_DV)
    cos, sin = rope_tables(jnp.arange(L, dtype=jnp.int32), RET_DK, RET_ROPE_BASE)
    log_gamma = jax.nn.log_sigmoid(logit_gamma.astype(f32))
    fp = lambda t: flip_parts(t, n_ctx)
    y_f = retention_scan(apply_rope(q, cos, sin), apply_rope(k, cos, sin), v, log_gamma[0])
    y_b = retention_scan(apply_rope(fp(q), cos, sin), apply_rope(fp(k), cos, sin), fp(v), log_gamma[1])
    y = y_f + fp(y_b)
    if not need_ctx:
        y, g = y[:, n_ctx:], g[:, n_ctx:]
    mu = jnp.mean(y, axis=-1, keepdims=True)
    var = jnp.mean(jnp.square(y - mu), axis=-1, keepdims=True)
    y = ((y - mu) * lax.rsqrt(var + NORM_EPS)).reshape(bsz, y.shape[1], v_w)
    y = (y * gn_w + gn_b) * jax.nn.silu(g.astype(f32))
    out = y.astype(h.dtype) @ w_out
    return split_ctx_lat(out, n_ctx, need_ctx)


def mlstm_mixer(h_ctx, h_lat, w_in, conv_w, b_gate, norm_w, w_out, need_ctx):
    n_ctx = h_ctx.shape[1]
    h = jnp.concatenate([h_ctx, h_lat], axis=1)
    bsz, L, _ = h.shape
    qk, v, o, gates = jnp.split(h @ w_in, [2 * MLSTM_QK, 2 * MLSTM_QK + MLSTM_V, 2 * MLSTM_QK + 2 * MLSTM_V], axis=-1)
    qk = jax.nn.silu(jnp.concatenate([dwconv(qk[:, :n_ctx], conv_w), dwconv(qk[:, n_ctx:], conv_w)], axis=1))
    q, k = jnp.split(qk, 2, axis=-1)
    q = q.reshape(bsz, L, MLSTM_HEADS, MLSTM_DK) * (MLSTM_DK ** -0.5)
    k = k.reshape(bsz, L, MLSTM_HEADS, MLSTM_DK)
    v = v.reshape(bsz, L, MLSTM_HEADS, MLSTM_DV)
    gates = gates.astype(f32).reshape(bsz, L, 2, 2, MLSTM_HEADS) + b_gate.astype(f32)
    i_pre = GATE_SOFTCAP * jnp.tanh(gates[:, :, :, 0] / GATE_SOFTCAP)
    log_f = jax.nn.log_sigmoid(gates[:, :, :, 1])
    fp = lambda t: flip_parts(t, n_ctx)
    y_f = mlstm_scan(q, k, v, i_pre[:, :, 0], log_f[:, :, 0])
    y_b = mlstm_scan(fp(q), fp(k), fp(v), fp(i_pre[:, :, 1]), fp(log_f[:, :, 1]))
    y = y_f + fp(y_b)
    if not need_ctx:
        y, o = y[:, n_ctx:], o[:, n_ctx:]
    y = y * lax.rsqrt(jnp.mean(jnp.square(y), axis=-1, keepdims=True) + NORM_EPS)
    y = y.reshape(bsz, y.shape[1], MLSTM_V) * norm_w * jax.nn.sigmoid(o.astype(f32))
    out = y.astype(h.dtype) @ w_out
    return split_ctx_lat(out, n_ctx, need_ctx)


def attend(qb, kb, vb):
    s = jnp.einsum('bqkgd,bskd->bkgqs', qb, kb).astype(f32) * (ATTN_HD ** -0.5)
    p = jax.nn.softmax(s, axis=-1)
    return jnp.einsum('bkgqs,bskd->bqkgd', p.astype(vb.dtype), vb)


def attention_mixer(h_ctx, h_lat, w_in, q_norm, k_norm, w_out, need_ctx):
    n_ctx, n_lat = h_ctx.shape[1], h_lat.shape[1]
    h = jnp.concatenate([h_ctx, h_lat], axis=1)
    bsz, L, _ = h.shape
    q, k, v = jnp.split(h @ w_in, [ATTN_HEADS * ATTN_HD, (ATTN_HEADS + ATTN_KV_HEADS) * ATTN_HD], axis=-1)
    q = rmsnorm(q.reshape(bsz, L, ATTN_HEADS, ATTN_HD), q_norm)
    k = rmsnorm(k.reshape(bsz, L, ATTN_KV_HEADS, ATTN_HD), k_norm)
    v = v.reshape(bsz, L, ATTN_KV_HEADS, ATTN_HD)
    tabs = axial_rope_tables(n_ctx, n_lat)
    q = apply_axial_rope(q, *tabs).reshape(bsz, L, ATTN_KV_HEADS, ATTN_GROUPS, ATTN_HD)
    k = apply_axial_rope(k, *tabs)
    n_blocks = n_lat // Q_BLOCK
    q_blocks = jnp.moveaxis(q[:, n_ctx:].reshape(bsz, n_blocks, Q_BLOCK, ATTN_KV_HEADS, ATTN_GROUPS, ATTN_HD), 1, 0)
    o_lat = lax.map(lambda qb: attend(qb, k, v), q_blocks)
    o_lat = jnp.moveaxis(o_lat, 0, 1).reshape(bsz, n_lat, D_MODEL)
    y_lat = o_lat @ w_out
    if not need_ctx:
        return None, y_lat
    o_ctx = attend(q[:, :n_ctx], k[:, :n_ctx], v[:, :n_ctx]).reshape(bsz, n_ctx, D_MODEL)
    return o_ctx @ w_out, y_lat


def hier_moe(h, w_rg, b_rg, w_re, b_re, w_gate, w_up, w_down):
    T, D = h.shape
    g_prob = jax.nn.softmax((h @ w_rg).astype(f32) + b_rg.astype(f32), axis=-1)
    g_w, g_idx = lax.top_k(g_prob, 1)
    e_logit = ((h @ w_re).astype(f32) + b_re.astype(f32)).reshape(T, MOE_GROUPS, MOE_PER_GROUP)
    e_logit = jnp.take_along_axis(e_logit, g_idx[:, :, None], axis=1)[:, 0]
    e_w, e_idx = lax.top_k(jax.nn.softmax(e_logit, axis=-1), MOE_TOPK)
    e_w = e_w / jnp.sum(e_w, axis=-1, keepdims=True)
    weight = (g_w * e_w).reshape(-1)
    expert = (g_idx * MOE_PER_GROUP + e_idx).reshape(-1)
    token = jnp.repeat(jnp.arange(T, dtype=jnp.int32), MOE_TOPK)
    n_assign = T * MOE_TOPK
    order = jnp.argsort(expert)
    e_sorted = expert[order]
    counts = jnp.bincount(expert, length=MOE_EXPERTS)
    padded = (counts + MOE_BLOCK - 1) // MOE_BLOCK * MOE_BLOCK
    seg_end = jnp.cumsum(padded)
    dest = (seg_end - padded)[e_sorted] + jnp.arange(n_assign, dtype=jnp.int32) - (jnp.cumsum(counts) - counts)[e_sorted]
    n_blocks = (n_assign + MOE_EXPERTS * (MOE_BLOCK - 1)) // MOE_BLOCK + 1
    slot_tok = jnp.zeros((n_blocks * MOE_BLOCK,), jnp.int32).at[dest].set(token[order])
    slot_w = jnp.zeros((n_blocks * MOE_BLOCK,), f32).at[dest].set(weight[order])
    block_expert = jnp.minimum(jnp.searchsorted(seg_end, jnp.arange(n_blocks, dtype=jnp.int32) * MOE_BLOCK, side='right'),
                               MOE_EXPERTS - 1)

    def expert_block(args):
        tok, w, e = args
        xb = h[tok]
        yb = (jax.nn.silu(xb @ w_gate[e]) * (xb @ w_up[e])) @ w_down[e]
        return yb * w[:, None].astype(yb.dtype)

    ys = lax.map(expert_block, (slot_tok.reshape(n_blocks, MOE_BLOCK), slot_w.reshape(n_blocks, MOE_BLOCK), block_expert))
    return jnp.zeros_like(h).at[slot_tok].add(ys.reshape(-1, D).astype(h.dtype))


def setup_inputs(seed: int = 0) -> dict:
    key = jax.random.key(seed)
    ks = iter(jax.random.split(key, 40))
    D = D_MODEL

    def nrm(shape, scale):
        return jax.random.normal(next(ks), shape, f32) * scale

    x = nrm((BATCH, SEQ, D), 1.0)
    c = nrm((BATCH, D), 1.0)
    ctx = nrm((BATCH, CTX_LEN, D), 1.0)
    c_ctx = nrm((D,), 1.0)
    w_ada = nrm((DEPTH, D, 6 * D), ADA_INIT * D ** -0.5)
    b_ada = nrm((DEPTH, 6 * D), 0.02)
    norm_g = 1.0 + nrm((DEPTH, 2, D), 0.02)
    ret_w_in = nrm((N_RET_LAYERS, D, RET_IN), D ** -0.5)
    base_logit = jnp.log(jnp.power(2.0, 5.0 + jnp.arange(RET_HEADS, dtype=f32)) - 1.0)
    ret_logit_gamma = base_logit + nrm((N_RET_LAYERS, 2, RET_HEADS), 0.05)
    ret_gn_w = 1.0 + nrm((N_RET_LAYERS, RET_HEADS * RET_DV), 0.02)
    ret_gn_b = nrm((N_RET_LAYERS, RET_HEADS * RET_DV), 0.02)
    ret_w_out = nrm((N_RET_LAYERS, RET_HEADS * RET_DV, D), (RET_HEADS * RET_DV) ** -0.5)
    mlstm_w_in = nrm((N_MLSTM_LAYERS, D, MLSTM_IN), D ** -0.5)
    mlstm_conv_w = nrm((N_MLSTM_LAYERS, MLSTM_CONV, 2 * MLSTM_QK), MLSTM_CONV ** -0.5)
    i_bias = nrm((N_MLSTM_LAYERS, 2, 1, MLSTM_HEADS), 0.1)
    f_bias = jnp.linspace(3.0, 6.0, MLSTM_HEADS, dtype=f32) + nrm((N_MLSTM_LAYERS, 2, 1, MLSTM_HEADS), 0.1)
    mlstm_b_gate = jnp.concatenate([i_bias, f_bias], axis=2)
    mlstm_norm_w = 1.0 + nrm((N_MLSTM_LAYERS, MLSTM_V), 0.02)
    mlstm_w_out = nrm((N_MLSTM_LAYERS, MLSTM_V, D), MLSTM_V ** -0.5)
    attn_w_in = nrm((N_ATTN_LAYERS, D, ATTN_IN), D ** -0.5)
    attn_q_norm = 1.0 + nrm((N_ATTN_LAYERS, ATTN_HD), 0.02)
    attn_k_norm = 1.0 + nrm((N_ATTN_LAYERS, ATTN_HD), 0.02)
    attn_w_out = nrm((N_ATTN_LAYERS, ATTN_HEADS * ATTN_HD, D), (ATTN_HEADS * ATTN_HD) ** -0.5)
    moe_w_router_group = nrm((DEPTH, D, MOE_GROUPS), D ** -0.5)
    moe_b_router_group = nrm((DEPTH, MOE_GROUPS), 0.01)
    moe_w_router_expert = nrm((DEPTH, D, MOE_EXPERTS), D ** -0.5)
    moe_b_router_expert = nrm((DEPTH, MOE_EXPERTS), 0.01)
    moe_w_gate = nrm((DEPTH, MOE_EXPERTS, D, MOE_FF), D ** -0.5)
    moe_w_up = nrm((DEPTH, MOE_EXPERTS, D, MOE_FF), D ** -0.5)
    moe_w_down = nrm((DEPTH, MOE_EXPERTS, MOE_FF, D), MOE_FF ** -0.5)
    final_norm_g = 1.0 + nrm((D,), 0.02)
    return {'x': x, 'c': c, 'ctx': ctx, 'c_ctx': c_ctx, 'w_ada': w_ada, 'b_ada': b_ada, 'norm_g': norm_g,
            'ret_w_in': ret_w_in, 'ret_logit_gamma': ret_logit_gamma, 'ret_gn_w': ret_gn_w, 'ret_gn_b': ret_gn_b,
            'ret_w_out': ret_w_out, 'mlstm_w_in': mlstm_w_in, 'mlstm_conv_w': mlstm_conv_w,
            'mlstm_b_gate': mlstm_b_gate, 'mlstm_norm_w': mlstm_norm_w, 'mlstm_w_out': mlstm_w_out,
            'attn_w_in': attn_w_in, 'attn_q_norm': attn_q_norm, 'attn_k_norm': attn_k_norm, 'attn_w_out': attn_w_out,
            'moe_w_router_group': moe_w_router_group, 'moe_b_router_group': moe_b_router_group,
            'moe_w_router_expert': moe_w_router_expert, 'moe_b_router_expert': moe_b_router_expert,
            'moe_w_gate': moe_w_gate, 'moe_w_up': moe_w_up, 'moe_w_down': moe_w_down, 'final_norm_g': final_norm_g}


def reference(x, c, ctx, c_ctx, w_ada, b_ada, norm_g, ret_w_in, ret_logit_gamma, ret_gn_w, ret_gn_b, ret_w_out,
              mlstm_w_in, mlstm_conv_w, mlstm_b_gate, mlstm_norm_w, mlstm_w_out, attn_w_in, attn_q_norm,
              attn_k_norm, attn_w_out, moe_w_router_group, moe_b_router_group, moe_w_router_expert,
              moe_b_router_expert, moe_w_gate, moe_w_up, moe_w_down, final_norm_g):
    n_ctx, n_lat = ctx.shape[1], x.shape[1]
    x_lat, x_ctx = x, ctx
    s_lat = jax.nn.silu(c)
    s_ctx = jax.nn.silu(c_ctx)
    for i in range(DEPTH):
        kind, j = i % N_MIXERS, i // N_MIXERS
        need_ctx = i < DEPTH - 1
        mod_lat = jnp.split((s_lat @ w_ada[i] + b_ada[i])[:, None, :], 6, axis=-1)
        mod_ctx = jnp.split((s_ctx @ w_ada[i] + b_ada[i])[None, None, :], 6, axis=-1)
        h_lat = modulate(rmsnorm(x_lat, norm_g[i, 0]), mod_lat[0], mod_lat[1])
        h_ctx = modulate(rmsnorm(x_ctx, norm_g[i, 0]), mod_ctx[0], mod_ctx[1])
        if kind == 0:
            y_ctx, y_lat = retention_mixer(h_ctx, h_lat, ret_w_in[j], ret_logit_gamma[j], ret_gn_w[j], ret_gn_b[j],
                                           ret_w_out[j], need_ctx)
        elif kind == 1:
            y_ctx, y_lat = mlstm_mixer(h_ctx, h_lat, mlstm_w_in[j], mlstm_conv_w[j], mlstm_b_gate[j], mlstm_norm_w[j],
                                       mlstm_w_out[j], need_ctx)
        else:
            y_ctx, y_lat = attention_mixer(h_ctx, h_lat, attn_w_in[j], attn_q_norm[j], attn_k_norm[j], attn_w_out[j],
                                           need_ctx)
        x_lat = x_lat + (mod_lat[2] * y_lat).astype(x_lat.dtype)
        if need_ctx:
            x_ctx = x_ctx + (mod_ctx[2] * y_ctx).astype(x_ctx.dtype)
        h_lat = modulate(rmsnorm(x_lat, norm_g[i, 1]), mod_lat[3], mod_lat[4])
        if need_ctx:
            h_ctx = modulate(rmsnorm(x_ctx, norm_g[i, 1]), mod_ctx[3], mod_ctx[4])
            h_all = jnp.concatenate([h_ctx, h_lat], axis=1)
        else:
            h_all = h_lat
        y_all = hier_moe(h_all.reshape(-1, D_MODEL), moe_w_router_group[i], moe_b_router_group[i],
                         moe_w_router_expert[i], moe_b_router_expert[i], moe_w_gate[i], moe_w_up[i],
                         moe_w_down[i]).reshape(h_all.shape)
        x_lat = x_lat + (mod_lat[5] * y_all[:, y_all.shape[1] - n_lat:]).astype(x_lat.dtype)
        if need_ctx:
            x_ctx = x_ctx + (mod_ctx[5] * y_all[:, :n_ctx]).astype(x_ctx.dtype)
    return rmsnorm(x_lat, final_norm_g)
```

```python
import numpy as np
from contextlib import ExitStack
import concourse.bass as bass
import concourse.mybir as mybir
from concourse.bass_utils import run_bass_kernel_spmd

F32 = mybir.dt.float32
BF16 = mybir.dt.bfloat16
I32 = mybir.dt.int32
ALU = mybir.AluOpType
AF = mybir.ActivationFunctionType
AX = mybir.AxisListType


class Trk:
    __slots__ = ("wr", "wdma", "rd", "rdma")

    def __init__(self):
        self.wr = {}
        self.wdma = []
        self.rd = {}
        self.rdma = []


class V:
    __slots__ = ("t", "ap")

    def __init__(self, t, ap):
        self.t = t
        self.ap = ap

    def __getitem__(self, k):
        return V(self.t, self.ap[k])

    def re(self, pat, **kw):
        return V(self.t, self.ap.rearrange(pat, **kw))

    def bc(self, shape):
        return V(self.t, self.ap.broadcast_to(shape))


class Op:
    __slots__ = ("eng", "fn", "waits", "signal", "sem", "val", "dma")

    def __init__(self, eng, fn, dma=False):
        self.eng = eng
        self.fn = fn
        self.waits = []
        self.signal = False
        self.sem = None
        self.val = 0
        self.dma = dma


ENGS = ("pe", "dve", "act", "pool", "sp")


class Prog:
    def __init__(self, nc, n_dma_sems=30, same_engine_sync=True):
        self.nc = nc
        self.es = ExitStack()
        self.ops = {e: [] for e in ENGS}
        self.same = same_engine_sync
        self.esem = {e: self.es.enter_context(nc.semaphore("s_" + e)) for e in ENGS}
        self.dsem = [self.es.enter_context(nc.semaphore("d%d" % i)) for i in range(n_dma_sems)]
        self.dcnt = [0] * n_dma_sems
        self.dlast = [None] * n_dma_sems
        self.dnext = 0
        self.dnext_sw = 0
        self.out_dmas = []
        self.nalloc = 0
        self.dma_since = []

    def sb(self, shape, dtype=F32, name=None):
        self.nalloc += 1
        t = self.es.enter_context(self.nc.sbuf_tensor(name or "sb%d" % self.nalloc, list(shape), dtype))
        return V(Trk(), t[:] if not hasattr(t, "ap") or True else t.ap())

    def ps(self, shape, dtype=F32, name=None):
        self.nalloc += 1
        t = self.es.enter_context(self.nc.psum_tensor(name or "ps%d" % self.nalloc, list(shape), dtype))
        return V(Trk(), t[:])

    def split(self, v, aps):
        return [V(Trk(), a) for a in aps]

    def dram_in(self, name, shape, dtype=F32):
        return V(Trk(), self.nc.dram_tensor(name, list(shape), dtype, kind="ExternalInput").ap())

    def dram_out(self, name, shape, dtype=F32):
        return V(Trk(), self.nc.dram_tensor(name, list(shape), dtype, kind="ExternalOutput").ap())

    def dram_tmp(self, name, shape, dtype=F32):
        return V(Trk(), self.nc.dram_tensor(name, list(shape), dtype, kind="Internal").ap())

    def _rec(self, eng, fn, reads, writes, dma=False):
        op = Op(eng, fn, dma)
        deps = []
        for r in reads:
            if r is None:
                continue
            deps.extend(r.t.wr.values())
            deps.extend(r.t.wdma)
        for w in writes:
            deps.extend(w.t.wr.values())
            deps.extend(w.t.wdma)
            deps.extend(w.t.rd.values())
            deps.extend(w.t.rdma)
        seen = set()
        for d in deps:
            if id(d) in seen or d is op:
                continue
            seen.add(id(d))
            if d.eng == eng and not d.dma:
                if eng in ("pe", "sp") or not self.same:
                    continue
            d.signal = True
            op.waits.append(d)
        for r in reads:
            if r is None:
                continue
            if dma:
                r.t.rdma.append(op)
                if len(r.t.rdma) > 16:
                    r.t.rdma.pop(0)
            else:
                r.t.rd[eng] = op
        for w in writes:
            w.t.rd = {}
            w.t.rdma = []
            if dma:
                w.t.wdma.append(op)
                if len(w.t.wdma) > 16:
                    w.t.wdma.pop(0)
            else:
                w.t.wr[eng] = op
        if dma:
            nq = 6
            if eng == "pool":
                k = self.dnext_sw
                self.dnext_sw = (self.dnext_sw + 1) % nq
            else:
                k = nq + self.dnext
                self.dnext = (self.dnext + 1) % (len(self.dsem) - nq)
            if self.dlast[k] is not None:
                op.waits.append(self.dlast[k])
            self.dcnt[k] += 16
            op.sem = self.dsem[k]
            op.val = self.dcnt[k]
            op.signal = True
            self.dlast[k] = op
            self.dma_since.append(op)
        self.ops[eng].append(op)
        return op

    def barrier(self):
        lasts = []
        for e in ENGS:
            for o in reversed(self.ops[e]):
                if o.fn is not None and not o.dma:
                    o.signal = True
                    lasts.append(o)
                    break
        dmas = list(self.dma_since)
        self.dma_since = []
        for e in ENGS:
            b = Op(e, None)
            b.waits = [o for o in lasts if o.eng != e] + dmas
            self.ops[e].append(b)

    def view(self, region, lo, hi, dtype=None, pat=None, **kw):
        ap = region.ap[:, lo:hi]
        if dtype is not None:
            ap = ap.bitcast(dtype)
        if pat is not None:
            ap = ap.rearrange(pat, **kw)
        return V(Trk(), ap)

    def op(self, eng, fn, r=(), w=()):
        return self._rec(eng, fn, list(r), list(w))

    def dma(self, out, in_, eng="sp", is_output=False, **kw):
        o, i = out.ap, in_.ap
        op = self._rec(eng, lambda e: e.dma_start(out=o, in_=i, **kw), [in_], [out], dma=True)
        if is_output:
            self.out_dmas.append(op)
        return op

    def mm(self, out, lhsT, rhs, start=True, stop=True):
        o, l, r = out.ap, lhsT.ap, rhs.ap
        rd = [lhsT, rhs] + ([] if start else [out])
        return self._rec("pe", lambda e: e.matmul(o, l, r, start=start, stop=stop), rd, [out])

    def tr(self, out, in_, ident):
        o, i, d = out.ap, in_.ap, ident.ap
        return self._rec("pe", lambda e: e.transpose(o, i, d), [in_, ident], [out])

    def act(self, out, in_, func, bias=None, scale=None, accum=None, eng="act"):
        o, i = out.ap, in_.ap
        kw = {}
        rd = [in_]
        if bias is not None:
            if isinstance(bias, V):
                kw["bias"] = bias.ap
                rd.append(bias)
            else:
                kw["bias"] = bias
        if scale is not None:
            if isinstance(scale, V):
                kw["scale"] = scale.ap
                rd.append(scale)
            else:
                kw["scale"] = scale
        wr = [out]
        if accum is not None:
            kw["accum_out"] = accum.ap
            wr.append(accum)
        return self._rec(eng, lambda e: e.activation(o, i, func, **kw), rd, wr)

    def tt(self, out, a, b, op, eng="dve"):
        o, x, y = out.ap, a.ap, b.ap
        return self._rec(eng, lambda e: e.tensor_tensor(o, x, y, op), [a, b], [out])

    def ts(self, out, a, s1, op0, s2=None, op1=None, accum=None, eng="dve"):
        o, x = out.ap, a.ap
        rd = [a]
        if isinstance(s1, V):
            rd.append(s1)
            s1 = s1.ap
        if isinstance(s2, V):
            rd.append(s2)
            s2 = s2.ap
        kw = {}
        wr = [out]
        if accum is not None:
            kw["accum_out"] = accum.ap
            wr.append(accum)
        if op1 is None:
            return self._rec(eng, lambda e: e.tensor_scalar(o, x, s1, None, op0, **kw), rd, wr)
        return self._rec(eng, lambda e: e.tensor_scalar(o, x, s1, s2, op0, op1, **kw), rd, wr)

    def stt(self, out, a, s, b, op0, op1, accum=None, eng="dve"):
        o, x, y = out.ap, a.ap, b.ap
        rd = [a, b]
        if isinstance(s, V):
            rd.append(s)
            s = s.ap
        kw = {}
        wr = [out]
        if accum is not None:
            kw["accum_out"] = accum.ap
            wr.append(accum)
        return self._rec(eng, lambda e: e.scalar_tensor_tensor(o, x, s, y, op0, op1, **kw), rd, wr)

    def copy(self, out, in_, eng="dve"):
        o, i = out.ap, in_.ap
        if eng == "act":
            return self._rec(eng, lambda e: e.copy(o, i), [in_], [out])
        return self._rec(eng, lambda e: e.tensor_copy(o, i), [in_], [out])

    def memset(self, out, val, eng="dve"):
        o = out.ap
        return self._rec(eng, lambda e: e.memset(o, val), [], [out])

    def reduce(self, out, in_, op, axis=AX.X, eng="dve"):
        o, i = out.ap, in_.ap
        return self._rec(eng, lambda e: e.tensor_reduce(o, i, axis, op), [in_], [out])

    def recip(self, out, in_):
        o, i = out.ap, in_.ap
        return self._rec("dve", lambda e: e.reciprocal(o, i), [in_], [out])

    def finish(self):
        nc = self.nc
        fin = Op("sp", None)
        fin.waits = list(self.out_dmas)
        for e in ENGS:
            c = 0
            for op in self.ops[e]:
                if op.dma:
                    continue
                if op.signal:
                    c += 1
                    op.sem = self.esem[e]
                    op.val = c
        self.ops["sp"].append(fin)
        emap = {"pe": "tensor", "dve": "vector", "act": "scalar", "pool": "gpsimd", "sp": "sync"}
        with nc.Block() as block:
            for e in ENGS:
                ops = self.ops[e]

                def body(engine, ops=ops):
                    waited = {}
                    for op in ops:
                        for d in op.waits:
                            key = id(d.sem)
                            if waited.get(key, 0) >= d.val:
                                continue
                            engine.wait_ge(d.sem, d.val)
                            waited[key] = d.val
                        if op.fn is None:
                            continue
                        ins = op.fn(engine)
                        if op.signal:
                            ins.then_inc(op.sem, 16 if op.dma else 1)

                getattr(block, emap[e])(body)
        self.es.close()
        n = {e: len(self.ops[e]) for e in ENGS}
        return n


T = 1056
NCTX = 32
TB = [(0, 352), (352, 704), (704, 1056)]
D = 2048
KD = 16
EPS = 1e-6


def make_ident(P, n=128):
    ident = P.sb([128, 128], F32)
    P.memset(ident, 1.0, eng="pool")
    ia = ident.ap
    P.op("pool", lambda e: e.affine_select(ia, ia, [[-1, 128]], ALU.is_equal, 0.0, base=0, channel_multiplier=1), r=[ident], w=[ident])
    return ident


def rms_bc(P, xT, ones_f, ps_list, rstd, tmpsq):
    for bi, (a, b) in enumerate(TB):
        ps = ps_list[bi % len(ps_list)]
        for k in range(KD):
            P.act(tmpsq[k % 2][:, 0:b - a], xT[:, k, a:b], AF.Square)
            P.mm(ps[:, 0:b - a], ones_f, tmpsq[k % 2][:, 0:b - a], start=(k == 0), stop=(k == KD - 1))
        P.ts(rstd[:, a:b], ps[:, 0:b - a], 1.0 / D, ALU.mult, EPS, ALU.add)
        P.act(rstd[:, a:b], rstd[:, a:b], AF.Sqrt)
        P.recip(rstd[:, a:b], rstd[:, a:b])


def modulate_T(P, out, xT, rstd, gs_lat, sh_lat, gs_ctx, sh_ctx, tmp):
    for k in range(KD):
        t = tmp[k % 2]
        P.tt(t, xT[:, k, :], rstd, ALU.mult)
        P.ts(out[:, k, 0:NCTX], t[:, 0:NCTX], gs_ctx[:, k:k + 1], ALU.mult, sh_ctx[:, k:k + 1], ALU.add)
        P.ts(out[:, k, NCTX:T], t[:, NCTX:T], gs_lat[:, k:k + 1], ALU.mult, sh_lat[:, k:k + 1], ALU.add, eng="pool")


def build_F(final, n_exp=32, dbg=False):
    nc = bass.Bass("TRN2", target_bir_lowering=False)
    P = Prog(nc)
    xT_d = P.dram_in("xT", [128, KD, T])
    NV = 12
    vec_d = P.dram_in("vec", [128, NV, KD])
    wr_d = P.dram_in("wr", [128, KD, 36])
    br_d = P.dram_in("br", [128, 36])
    wg_d = P.dram_in("wg", [n_exp, D, 768])
    wu_d = P.dram_in("wu", [n_exp, D, 768])
    wd_d = P.dram_in("wd", [n_exp, 768, D])
    xo_d = P.dram_out("xo", [128, KD, T])
    ho_d = P.dram_out("ho", [128, KD, T], F32 if final else BF16)

    xT = P.sb([128, KD, T], F32)
    hb = P.sb([128, KD, T], BF16)
    vec = P.sb([128, NV, KD], F32)
    gs = P.sb([128, 4, KD], F32)
    wr = P.sb([128, KD, 36], F32)
    br = P.sb([128, 36], F32)
    ones_f = P.sb([128, 128], F32)
    rstd = P.sb([128, T], F32)
    tmpsq = [P.sb([128, 352], F32) for _ in range(2)]
    tmpT = [P.sb([128, T], F32) for _ in range(2)]
    ident = make_ident(P)
    wgb = [P.sb([128, KD, 256], BF16) for _ in range(2)]
    wub = [P.sb([128, KD, 256], BF16) for _ in range(2)]
    wdb = [P.sb([128, 6, 512], BF16) for _ in range(2)]
    Ab = [[P.sb([128, T], BF16) for _ in range(6)]] * 2
    s_sb = [P.sb([128, 352], F32) for _ in range(2)]
    u_sb = [P.sb([128, 352], F32) for _ in range(2)]
    cwbc = [P.sb([128, T], F32) for _ in range(2)]
    ps_g = [P.ps([128, 512], F32) for _ in range(2)]
    ps_u = [P.ps([128, 512], F32) for _ in range(2)]
    ps_y = [P.ps([128, 512], F32) for _ in range(2)]
    ps_c = P.ps([128, 512], F32)
    ps_r = P.ps([128, 512], F32)

    for k in range(0, KD, 4):
        P.dma(xT[:, k:k + 4, :], xT_d[:, k:k + 4, :])
    P.dma(vec, vec_d)
    P.dma(wr, wr_d)
    P.dma(br, br_d)
    P.memset(ones_f, 1.0)
    def mk_gs(o, g, sc):
        P.ts(gs[:, o, :], vec[:, sc, :], 1.0, ALU.add)
        P.tt(gs[:, o, :], gs[:, o, :], vec[:, g, :], ALU.mult)
    mk_gs(0, 0, 2); mk_gs(1, 0, 5)
    if not final:
        mk_gs(2, 7, 9); mk_gs(3, 7, 11)

    rms_bc(P, xT, ones_f, [ps_g[0], ps_u[0], ps_y[0]], rstd, tmpsq)
    tts = [(i * 128, min(T, (i + 1) * 128)) for i in range(9)]
    psl = [ps_g[0], ps_u[0], ps_y[0]]
    for k in range(KD):
        t = tmpT[k % 2]
        P.tt(t, xT[:, k, :], rstd, ALU.mult)
        P.ts(t[:, 0:NCTX], t[:, 0:NCTX], gs[:, 1, k:k + 1], ALU.mult, vec[:, 4, k:k + 1], ALU.add)
        P.ts(t[:, NCTX:T], t[:, NCTX:T], gs[:, 0, k:k + 1], ALU.mult, vec[:, 1, k:k + 1], ALU.add)
        P.copy(hb[:, k, :], t, eng="pool")
        for bi, (a, b) in enumerate(TB):
            P.mm(psl[bi][0:36, 0:b - a], wr[:, k, :], t[:, a:b], start=(k == 0), stop=(k == KD - 1))
    NT = 9
    lg = P.sb([128, NT, 36], F32)
    P.memset(lg, 0.0)
    lgT = P.sb([36, T], F32)
    for bi, (a, b) in enumerate(TB):
        P.copy(lgT[:, a:b], psl[bi][0:36, 0:b - a])
    for i, (a, b) in enumerate(tts):
        P.tr(ps_r[0:b - a, 0:36], lgT[:, a:b], ident[0:36, 0:36])
        P.tt(lg[0:b - a, i, :], ps_r[0:b - a, 0:36], br[0:b - a, :], ALU.add)
    gl = lg[:, :, 0:4]
    gmax = P.sb([128, NT, 1], F32)
    P.reduce(gmax[:, :, 0], gl, ALU.max)
    gm = P.sb([128, NT, 4], F32)
    P.tt(gm, gl, gmax.bc([128, NT, 4]), ALU.is_ge)
    gex = P.sb([128, NT, 4], F32)
    P.tt(gex, gl, gmax.bc([128, NT, 4]), ALU.subtract)
    P.act(gex, gex, AF.Exp)
    gsum = P.sb([128, NT, 1], F32)
    P.reduce(gsum[:, :, 0], gex, ALU.add)
    gw = P.sb([128, NT, 1], F32)
    P.recip(gw, gsum)
    el = P.sb([128, NT, 4, 8], F32)
    pen = P.sb([128, NT, 4], F32)
    P.ts(pen, gm, -1.0, ALU.add, 1e30, ALU.mult)
    P.tt(el, lg[:, :, 4:36].re("p n (g e) -> p n g e", g=4), pen.ap and V(pen.t, pen.ap.unsqueeze(3).broadcast_to([128, NT, 4, 8])), ALU.add)
    elf = el.re("p n g e -> p n (g e)")
    m1 = P.sb([128, NT, 1], F32)
    P.reduce(m1[:, :, 0], elf, ALU.max)
    is1 = P.sb([128, NT, 32], F32)
    P.tt(is1, elf, m1.bc([128, NT, 32]), ALU.is_ge)
    e2 = P.sb([128, NT, 32], F32)
    P.stt(e2, is1, -1e30, elf, ALU.mult, ALU.add)
    m2 = P.sb([128, NT, 1], F32)
    P.reduce(m2[:, :, 0], e2, ALU.max)
    sel = P.sb([128, NT, 32], F32)
    P.tt(sel, elf, m2.bc([128, NT, 32]), ALU.is_ge)
    ex = P.sb([128, NT, 32], F32)
    P.tt(ex, elf, m1.bc([128, NT, 32]), ALU.subtract)
    P.ts(ex, ex, -80.0, ALU.max)
    P.act(ex, ex, AF.Exp)
    P.tt(ex, ex, sel, ALU.mult)
    den = P.sb([128, NT, 1], F32)
    P.reduce(den[:, :, 0], ex, ALU.add)
    P.recip(den, den)
    P.tt(den, den, gw, ALU.mult)
    cw = P.sb([128, NT, 32], F32)
    P.tt(cw, ex, den.bc([128, NT, 32]), ALU.mult)
    if dbg:
        lg_d = P.dram_out('lg_o', [128, NT, 36]); cw_d = P.dram_out('cw_o', [128, NT, 32])
        P.dma(lg_d, lg, is_output=True); P.dma(cw_d, cw, is_output=True)
    cwT = P.sb([32, T], F32)
    for i, (a, b) in enumerate(tts):
        P.tr(ps_c[0:32, 0:b - a], cw[0:b - a, i, :], ident[0:b - a, 0:b - a])
        P.copy(cwT[:, a:b], ps_c[0:32, 0:b - a])
    selm = [P.sb([32, 128], F32) for _ in range(2)]

    step_gu = 0
    step_d = 0
    pending = []

    def load_gu(e, j):
        nonlocal step_gu
        bsel = step_gu % 2
        step_gu += 1
        P.dma(wgb[bsel], wg_d[e, :, j * 256:(j + 1) * 256].re("(k p) f -> p k f", p=128), eng="pool")
        P.dma(wub[bsel], wu_d[e, :, j * 256:(j + 1) * 256].re("(k p) f -> p k f", p=128), eng="pool")
        return bsel

    def load_d(e, q):
        nonlocal step_d
        bsel = step_d % 2
        step_d += 1
        P.dma(wdb[bsel], wd_d[e, :, q * 512:(q + 1) * 512].re("(f p) d -> p f d", p=128), eng="pool")
        return bsel

    rot = 0
    roty = 0
    for e in range(n_exp):
        A = Ab[e % 2]
        cb = cwbc[e % 2]
        P.copy(selm[e % 2], ident[0:32, e:e + 1].bc([32, 128]))
        for bi, (a, b) in enumerate(TB):
            P.mm(ps_c[:, 0:b - a], selm[e % 2], cwT[:, a:b])
            P.copy(cb[:, a:b], ps_c[:, 0:b - a], eng="act")
        for j in range(3):
            bsel = load_gu(e, j)
            for fc in range(2):
                f = j * 2 + fc
                for bi, (a, b) in enumerate(TB):
                    G = ps_g[rot % 2]
                    U = ps_u[rot % 2]
                    s = s_sb[rot % 2]
                    u = u_sb[rot % 2]
                    rot += 1
                    n = b - a
                    for k in range(KD):
                        P.mm(G[:, 0:n], wgb[bsel][:, k, fc * 128:(fc + 1) * 128], hb[:, k, a:b], start=(k == 0), stop=(k == KD - 1))
                    for k in range(KD):
                        P.mm(U[:, 0:n], wub[bsel][:, k, fc * 128:(fc + 1) * 128], hb[:, k, a:b], start=(k == 0), stop=(k == KD - 1))
                    P.act(s[:, 0:n], G[:, 0:n], AF.Silu)
                    P.tt(u[:, 0:n], U[:, 0:n], cb[:, a:b], ALU.mult)
                    P.tt(A[f][:, a:b], s[:, 0:n], u[:, 0:n], ALU.mult, eng="pool")
        for q in range(4):
            bsel = load_d(e, q)
            for mc in range(4):
                m = q * 4 + mc
                for bi, (a, b) in enumerate(TB):
                    Y = ps_y[roty % 2]
                    roty += 1
                    n = b - a
                    for f in range(6):
                        P.mm(Y[:, 0:n], wdb[bsel][:, f, mc * 128:(mc + 1) * 128], A[f][:, a:b], start=(f == 0), stop=(f == 5))
                    if bi == 0:
                        P.stt(xT[:, m, 0:NCTX], Y[:, 0:NCTX], vec[:, 6, m:m + 1], xT[:, m, 0:NCTX], ALU.mult, ALU.add)
                        P.stt(xT[:, m, NCTX:b], Y[:, NCTX:n], vec[:, 3, m:m + 1], xT[:, m, NCTX:b], ALU.mult, ALU.add)
                    else:
                        P.stt(xT[:, m, a:b], Y[:, 0:n], vec[:, 3, m:m + 1], xT[:, m, a:b], ALU.mult, ALU.add)

    for k in range(0, KD, 4):
        P.dma(xo_d[:, k:k + 4, :], xT[:, k:k + 4, :], is_output=True)
    rms_bc(P, xT, ones_f, [ps_g[0], ps_u[0], ps_y[0]], rstd, tmpsq)
    if final:
        for k in range(KD):
            t = tmpT[k % 2]
            P.tt(t, xT[:, k, :], rstd, ALU.mult)
            P.ts(t, t, vec[:, 7, k:k + 1], ALU.mult)
            P.dma(ho_d[:, k, :], t, is_output=True)
    else:
        modulate_T(P, hb, xT, rstd, gs[:, 2, :], vec[:, 8, :], gs[:, 3, :], vec[:, 10, :], tmpT)
        for k in range(0, KD, 4):
            P.dma(ho_d[:, k:k + 4, :], hb[:, k:k + 4, :], is_output=True)
    P.finish()
    return nc


def build_ADA():
    nc = bass.Bass("TRN2", target_bir_lowering=False)
    P = Prog(nc)
    cs_d = P.dram_in("cs", [128, KD, 2])
    w_d = P.dram_in("w", [4, D, 1536])
    b_d = P.dram_in("b", [2, 4, 1536])
    o_d = P.dram_out("mod", [2, 4, 1536])
    cs = P.sb([128, KD, 2], F32)
    bs = P.sb([2, 4, 1536], F32)
    os_ = P.sb([2, 4, 1536], F32)
    wt = [P.sb([128, 8, 1536], F32) for _ in range(2)]
    ps = [P.ps([128, 512], F32) for _ in range(3)]
    P.dma(cs, cs_d)
    P.dma(bs, b_d)
    P.act(cs, cs, AF.Silu)
    step = 0
    for l in range(4):
        for h in range(2):
            w = wt[step % 2]
            step += 1
            P.dma(w, w_d[l, h * 1024:(h + 1) * 1024, :].re("(k p) n -> p k n", p=128))
            for kk in range(8):
                k = h * 8 + kk
                for nb in range(3):
                    P.mm(ps[nb][0:2, :], cs[:, k, :], w[:, kk, nb * 512:(nb + 1) * 512], start=(k == 0), stop=(k == KD - 1))
        for nb in range(3):
            P.tt(os_[:, l, nb * 512:(nb + 1) * 512], ps[nb][0:2, :], bs[:, l, nb * 512:(nb + 1) * 512], ALU.add)
    P.dma(o_d, os_, is_output=True)
    P.finish()
    return nc


def build_P(KZ):
    nc = bass.Bass("TRN2", target_bir_lowering=False)
    P = Prog(nc)
    xT_d = P.dram_in("xT", [128, KD, T])
    zT_d = P.dram_in("zT", [128, KZ, T], BF16)
    w_d = P.dram_in("w", [KZ * 128, D])
    g_d = P.dram_in("gate", [128, 2, KD])
    xo_d = P.dram_out("xo", [128, KD, T])
    xT = P.sb([128, KD, T], F32)
    zT = P.sb([128, KZ, T], BF16)
    gate = P.sb([128, 2, KD], F32)
    wq = [P.sb([128, KZ, 512], BF16) for _ in range(2)]
    ps = [P.ps([128, 512], F32) for _ in range(4)]
    for k in range(0, KD, 4):
        P.dma(xT[:, k:k + 4, :], xT_d[:, k:k + 4, :])
    for k in range(0, KZ, 4):
        P.dma(zT[:, k:k + 4, :], zT_d[:, k:k + 4, :])
    P.dma(gate, g_d)
    rot = 0
    for q in range(4):
        w = wq[q % 2]
        P.dma(w, w_d[:, q * 512:(q + 1) * 512].re("(k p) n -> p k n", p=128), eng="pool")
        for mc in range(4):
            m = q * 4 + mc
            for bi, (a, b) in enumerate(TB):
                Y = ps[rot % 4]
                rot += 1
                n = b - a
                for k in range(KZ):
                    P.mm(Y[:, 0:n], w[:, k, mc * 128:(mc + 1) * 128], zT[:, k, a:b], start=(k == 0), stop=(k == KZ - 1))
                if bi == 0:
                    P.stt(xT[:, m, 0:NCTX], Y[:, 0:NCTX], gate[:, 1, m:m + 1], xT[:, m, 0:NCTX], ALU.mult, ALU.add)
                    P.stt(xT[:, m, NCTX:b], Y[:, NCTX:n], gate[:, 0, m:m + 1], xT[:, m, NCTX:b], ALU.mult, ALU.add)
                else:
                    P.stt(xT[:, m, a:b], Y[:, 0:n], gate[:, 0, m:m + 1], xT[:, m, a:b], ALU.mult, ALU.add)
            if mc == 3 and q % 1 == 0:
                pass
    for k in range(0, KD, 4):
        P.dma(xo_d[:, k:k + 4, :], xT[:, k:k + 4, :], is_output=True)
    P.finish()
    return nc


L = 8448
NCH = 66
EPS = 1e-6


def blocks(n, step=512):
    return [(a, min(n, a + step)) for a in range(0, n, step)]


def build_ATT():
    nc = bass.Bass("TRN2", target_bir_lowering=False)
    P = Prog(nc)
    hT_d = P.dram_in("hT", [128, 16, L], BF16)
    w_d = P.dram_in("w", [2048, 512])
    nw_d = P.dram_in("nw", [128, 2])
    cos_d = P.dram_in("cosT", [128, L])
    sin_d = P.dram_in("sinT", [128, L])
    R_d = P.dram_in("RT", [128, 128])
    z_d = P.dram_out("zT", [128, 2, L], BF16)

    w = P.sb([128, 16, 512], BF16)
    nw = P.sb([128, 2], F32)
    RT = P.sb([128, 128], F32)
    ones_f = P.sb([128, 128], F32)
    ones_b = P.sb([128, 128], BF16)
    hb = [P.sb([128, 16, 512], BF16) for _ in range(2)]
    cosb = [P.sb([128, 512], F32) for _ in range(2)]
    sinb = [P.sb([128, 512], F32) for _ in range(2)]
    qT = P.sb([128, 2, L], BF16)
    kT = P.sb([128, L], BF16)
    Vt = P.sb([128, NCH, 128], BF16)
    zT = [P.sb([128, L], BF16) for _ in range(2)]
    sq = [P.sb([128, 512], F32) for _ in range(2)]
    rs = [P.sb([128, 512], F32) for _ in range(2)]
    qn = [P.sb([128, 512], F32) for _ in range(2)]
    t1 = [P.sb([128, 512], F32) for _ in range(2)]
    t2 = [P.sb([128, 512], F32) for _ in range(2)]
    pT = [P.sb([128, 512], BF16) for _ in range(3)]
    rl = P.sb([128, 512], F32)
    ps = [P.ps([128, 512], F32) for _ in range(8)]

    P.dma(w, w_d.re("(k p) n -> p k n", p=128), eng="pool")
    P.dma(nw, nw_d)
    P.dma(RT, R_d)
    P.memset(ones_f, 1.0)
    P.memset(ones_b, 1.0)

    it = 0
    for bi, (a, b) in enumerate(blocks(L)):
        n = b - a
        h = hb[bi % 2]
        P.dma(h[:, :, 0:n], hT_d[:, :, a:b])
        P.dma(cosb[bi % 2][:, 0:n], cos_d[:, a:b])
        P.dma(sinb[bi % 2][:, 0:n], sin_d[:, a:b])
        for j in range(3):
            pq = ps[it % 2]
            p2 = ps[2 + it % 2]
            p3 = ps[4 + it % 2]
            s_, r_, q_, a_, b_ = sq[it % 2], rs[it % 2], qn[it % 2], t1[it % 2], t2[it % 2]
            it += 1
            for k in range(16):
                P.mm(pq[:, 0:n], w[:, k, j * 128:(j + 1) * 128], h[:, k, 0:n], start=(k == 0), stop=(k == 15))
            P.act(s_[:, 0:n], pq[:, 0:n], AF.Square)
            P.mm(p2[:, 0:n], ones_f, s_[:, 0:n])
            P.ts(r_[:, 0:n], p2[:, 0:n], 1.0 / 128, ALU.mult, EPS, ALU.add)
            P.act(r_[:, 0:n], r_[:, 0:n], AF.Sqrt)
            P.recip(r_[:, 0:n], r_[:, 0:n])
            nwc = nw[:, 0:1] if j < 2 else nw[:, 1:2]
            P.stt(q_[:, 0:n], pq[:, 0:n], nwc, r_[:, 0:n], ALU.mult, ALU.mult)
            P.mm(p3[:, 0:n], RT, q_[:, 0:n])
            P.tt(a_[:, 0:n], q_[:, 0:n], cosb[bi % 2][:, 0:n], ALU.mult, eng="pool")
            P.tt(b_[:, 0:n], p3[:, 0:n], sinb[bi % 2][:, 0:n], ALU.mult)
            dst = qT[:, j, a:b] if j < 2 else kT[:, a:b]
            P.tt(dst, a_[:, 0:n], b_[:, 0:n], ALU.add, eng="pool")
        pv = ps[6 + bi % 2]
        nt = n // 128
        for ti in range(nt):
            for k in range(16):
                P.mm(pv[:, ti * 128:(ti + 1) * 128], h[:, k, ti * 128:(ti + 1) * 128], w[:, k, 384:512], start=(k == 0), stop=(k == 15))
        c0 = a // 128
        P.copy(Vt[:, c0:c0 + nt, :], pv[:, 0:nt * 128].re("p (c d) -> p c d", d=128), eng="act")

    scale = 128 ** -0.5
    rot = 0
    ro = 0
    for hq in range(2):
        qblocks = [(0, 256, 2)] + [(256 + i * 512, 256 + (i + 1) * 512, NCH) for i in range(16)]
        for (a, b, nkc) in qblocks:
            n = b - a
            po = ps[4 + ro % 2]
            pl = ps[6 + ro % 2]
            ro += 1
            for c in range(nkc):
                pS = ps[rot % 3]
                pt = pT[rot % 3]
                rot += 1
                P.mm(pS[:, 0:n], kT[:, c * 128:(c + 1) * 128], qT[:, hq, a:b])
                P.act(pt[:, 0:n], pS[:, 0:n], AF.Exp, scale=scale)
                P.mm(po[:, 0:n], Vt[:, c, :], pt[:, 0:n], start=(c == 0), stop=(c == nkc - 1))
                P.mm(pl[:, 0:n], ones_b, pt[:, 0:n], start=(c == 0), stop=(c == nkc - 1))
            P.recip(rl[:, 0:n], pl[:, 0:n])
            P.tt(zT[hq][:, a:b], po[:, 0:n], rl[:, 0:n], ALU.mult)
        for (a, b) in blocks(L, 2112):
            P.dma(z_d[:, hq, a:b], zT[hq][:, a:b], is_output=True)
    P.finish()
    return nc


def make_ident_f(P):
    ident = P.sb([128, 128], F32)
    P.memset(ident, 1.0, eng="pool")
    ia = ident.ap
    P.op("pool", lambda e: e.affine_select(ia, ia, [[-1, 128]], ALU.is_equal, 0.0, base=0, channel_multiplier=1), r=[ident], w=[ident])
    return ident


def build_RET():
    nc = bass.Bass("TRN2", target_bir_lowering=False)
    P = Prog(nc)
    hT_d = P.dram_in("hT", [128, 16, L], BF16)
    w_d = P.dram_in("w", [2048, 1536])
    lg_d = P.dram_in("lgam", [128, 2])
    gn_d = P.dram_in("gn", [128, 2, 512])
    tab_d = P.dram_in("tab", [4, 128, L])
    cst_d = P.dram_in("cst", [128, 6 * 128 + 4])
    z_d = P.dram_out("zT", [128, 4, L], BF16)
    v_s = P.dram_tmp("v_s", [NCH, 128, 512], BF16)
    g_s = P.dram_tmp("g_s", [NCH, 128, 512], BF16)
    y_s = P.dram_tmp("y_s", [NCH, 128, 512], F32)

    w = P.sb([128, 16, 1536], BF16)
    lg = P.sb([128, 2], F32)
    gn = P.sb([128, 2, 512], F32)
    cst = P.sb([128, 6 * 128 + 4], F32)
    qT = P.sb([128, 2, L], BF16)
    kT = P.sb([128, 2, L], BF16)
    hb = [P.sb([128, 16, 512], BF16)] * 2
    vst = [P.sb([128, 4, 512], BF16)] * 2
    gst = [P.sb([128, 4, 512], BF16)] * 2
    ident = make_ident_f(P)
    ident_b = P.sb([128, 128], BF16)
    P.copy(ident_b, ident)
    ps = [P.ps([128, 512], F32) for _ in range(6)]
    psb = [P.ps([128, 1024], BF16) for _ in range(2)]

    P.dma(w[:, 0:8, :], w_d[0:1024, :].re("(k p) n -> p k n", p=128), eng="pool")
    P.dma(w[:, 8:16, :], w_d[1024:2048, :].re("(k p) n -> p k n", p=128), eng="pool")
    P.dma(lg, lg_d)
    P.dma(gn, gn_d)
    P.dma(cst, cst_d)

    lgm = P.sb([128, 2], F32)
    P.act(lgm, lg, AF.Exp, scale=-1.0)
    P.ts(lgm, lgm, 1.0, ALU.add)
    P.act(lgm, lgm, AF.Ln)
    P.ts(lgm, lgm, -1.0, ALU.mult)
    dec = [P.sb([128, 128], F32) for _ in range(2)]
    qd = [P.sb([128, 128], F32) for _ in range(2)]
    kdc = P.sb([128, 2], F32)
    cdc = P.sb([128, 2], F32)
    for d in range(2):
        E = cst[:, (2 * d) * 128:(2 * d + 1) * 128]
        M = cst[:, (2 * d + 1) * 128:(2 * d + 2) * 128]
        X = cst[:, (4 + d) * 128:(5 + d) * 128]
        P.act(dec[d], E, AF.Exp, scale=lgm[:, d:d + 1])
        P.tt(dec[d], dec[d], M, ALU.mult)
        P.act(qd[d], X, AF.Exp, scale=lgm[:, d:d + 1])
        P.act(kdc[:, d:d + 1], cst[:, 768 + d:769 + d], AF.Exp, scale=lgm[:, d:d + 1])
        P.act(cdc[:, d:d + 1], cst[:, 770:771], AF.Exp, scale=lgm[:, d:d + 1])

    it = 0
    for bi, (a, b) in enumerate(blocks(L)):
        n = b - a
        nt = n // 128
        h = hb[bi % 2]
        P.dma(h[:, :, 0:n], hT_d[:, :, a:b])
        for j in range(4):
            pq = ps[it % 2]
            it += 1
            for k in range(16):
                P.mm(pq[:, 0:n], w[:, k, j * 128:(j + 1) * 128], h[:, k, 0:n], start=(k == 0), stop=(k == 15))
            if j < 2:
                P.act(qT[:, j, a:b], pq[:, 0:n], AF.Copy, scale=1.0 / 16.0)
            else:
                P.copy(kT[:, j - 2, a:b], pq[:, 0:n])
        vs, gs_ = vst[bi % 2], gst[bi % 2]
        for ti in range(nt):
            pv = ps[2 + ti % 2]
            for k in range(16):
                P.mm(pv, h[:, k, ti * 128:(ti + 1) * 128], w[:, k, 512:1024], start=(k == 0), stop=(k == 15))
            P.copy(vs[:, ti, :], pv, eng="act")
            pg = ps[4 + ti % 2]
            for k in range(16):
                P.mm(pg, h[:, k, ti * 128:(ti + 1) * 128], w[:, k, 1024:1536], start=(k == 0), stop=(k == 15))
            P.copy(gs_[:, ti, :], pg, eng="pool" if False else "dve")
        c0 = a // 128
        P.dma(v_s[c0:c0 + nt].re("c p n -> p c n"), vs[:, 0:nt, :])
        P.dma(g_s[c0:c0 + nt].re("c p n -> p c n"), gs_[:, 0:nt, :])

    S32 = P.sb([128, 2, 512], F32)
    Sb = [P.sb([128, 2, 512], BF16) for _ in range(2)]
    cs = [P.sb([128, 2, 128], F32) for _ in range(2)]
    vch = [P.sb([128, 512], BF16) for _ in range(3)]
    gch = [P.sb([128, 512], BF16) for _ in range(2)]
    yfb = [P.sb([128, 512], F32) for _ in range(2)]
    ta = [P.sb([128, 128], F32) for _ in range(4)]
    qr = [P.sb([128, 2, 128], BF16) for _ in range(2)]
    kr = [P.sb([128, 2, 128], BF16) for _ in range(2)]
    qdd = [P.sb([128, 2, 128], BF16) for _ in range(2)]
    sT = [P.sb([128, 128], BF16) for _ in range(2)]
    kd = [P.sb([128, 256], BF16) for _ in range(2)]
    yo = [P.sb([128, 512], F32) for _ in range(2)]
    yc = [P.sb([128, 512], F32) for _ in range(2)]
    st = [P.sb([128, 4], F32) for _ in range(2)]
    zb = [P.sb([128, 512], BF16) for _ in range(2)]
    sg = [P.sb([128, 512], F32) for _ in range(2)]
    zo = [P.sb([128, 4, 128], BF16) for _ in range(2)]

    def rope(dst, src, a, cs_):
        x1, x2 = src[:, 0, a:a + 128], src[:, 1, a:a + 128]
        c_, s_ = cs_[:, 0, :], cs_[:, 1, :]
        P.tt(ta[0], x1, c_, ALU.mult)
        P.tt(ta[1], x2, s_, ALU.mult)
        P.tt(dst[:, 0, :], ta[0], ta[1], ALU.subtract)
        P.tt(ta[2], x1, s_, ALU.mult, eng="pool")
        P.tt(ta[3], x2, c_, ALU.mult, eng="pool")
        P.tt(dst[:, 1, :], ta[2], ta[3], ALU.add, eng="pool")

    step = 0
    for d in range(2):
        order = list(range(NCH)) if d == 0 else [1, 0] + list(range(NCH - 1, 1, -1))
        P.memset(S32, 0.0)
        P.memset(Sb[step % 2], 0.0)
        for c in order:
            a = c * 128
            cur, nxt = Sb[step % 2], Sb[(step + 1) % 2]
            i2 = step % 2
            cs_ = cs[i2]
            P.dma(cs_, tab_d[2 * d:2 * d + 2, :, a:a + 128].re("t p n -> p t n"))
            v = vch[step % 3]
            P.dma(v, v_s[c])
            rope(qr[i2], qT, a, cs_)
            rope(kr[i2], kT, a, cs_)
            P.tt(qdd[i2], qr[i2], V(qd[d].t, qd[d].ap.unsqueeze(1).broadcast_to([128, 2, 128])), ALU.mult)
            pS = ps[0]
            for cc in range(2):
                P.mm(pS[:, 0:128], kr[i2][:, cc, :], qr[i2][:, cc, :], start=(cc == 0), stop=(cc == 1))
            P.tt(sT[i2], pS[:, 0:128], dec[d], ALU.mult)
            pK = psb[0]
            for cc in range(2):
                P.tr(pK[:, cc * 128:(cc + 1) * 128], kr[i2][:, cc, :], ident_b)
            P.ts(kd[i2], pK[:, 0:256], kdc[:, d:d + 1], ALU.mult)
            pO = ps[2 + step % 2]
            P.mm(pO, sT[i2], v, start=True, stop=False)
            for cc in range(2):
                P.mm(pO, qdd[i2][:, cc, :], cur[:, cc, :], start=False, stop=(cc == 1))
            for cc in range(2):
                pU = ps[4 + cc]
                P.mm(pU, kd[i2][:, cc * 128:(cc + 1) * 128], v)
                P.stt(S32[:, cc, :], S32[:, cc, :], cdc[:, d:d + 1], pU, ALU.mult, ALU.add)
                P.copy(nxt[:, cc, :], S32[:, cc, :], eng="act")
            if d == 0:
                y = yo[i2]
                P.copy(y, pO, eng="act")
                P.dma(y_s[c], y)
            else:
                yf = yfb[i2]
                g = gch[i2]
                P.dma(yf, y_s[c])
                P.dma(g, g_s[c])
                y = yo[i2]
                P.tt(y, pO, yf, ALU.add)
                s_ = st[i2]
                P.reduce(s_[:, 0:1], y, ALU.add)
                P.ts(s_[:, 1:2], s_[:, 0:1], 1.0 / 512, ALU.mult)
                ycc = yc[i2]
                P.ts(ycc, y, s_[:, 1:2], ALU.subtract)
                P.tt(sg[i2], ycc, ycc, ALU.mult, eng="pool")
                P.reduce(s_[:, 2:3], sg[i2], ALU.add)
                P.ts(s_[:, 3:4], s_[:, 2:3], 1.0 / 512, ALU.mult, EPS, ALU.add)
                P.act(s_[:, 3:4], s_[:, 3:4], AF.Sqrt)
                P.recip(s_[:, 3:4], s_[:, 3:4])
                P.stt(ycc, ycc, s_[:, 3:4], gn[:, 0, :], ALU.mult, ALU.mult)
                P.tt(ycc, ycc, gn[:, 1, :], ALU.add, eng="pool")
                P.act(sg[i2], g, AF.Silu)
                P.tt(zb[i2], ycc, sg[i2], ALU.mult)
                pT = psb[1]
                for vc in range(4):
                    P.tr(pT[:, vc * 128:(vc + 1) * 128], zb[i2][:, vc * 128:(vc + 1) * 128], ident_b)
                P.copy(zo[i2], pT[:, 0:512].re("p (c n) -> p c n", n=128), eng="act")
                P.dma(z_d[:, :, a:a + 128], zo[i2], is_output=True)
            step += 1
    P.finish()
    return nc


def build_MLS(stages=("A", "conv", "gate", "B")):
    nc = bass.Bass("TRN2", target_bir_lowering=False)
    P = Prog(nc)
    hT_d = P.dram_in("hT", [128, 16, L], BF16)
    wg_d = P.dram_in("wg", [2048, 4])
    w_d = P.dram_in("w", [2048, 768])
    cw_d = P.dram_in("cw", [128, 2, 3])
    bg_d = P.dram_in("bg", [128, 4])
    nw_d = P.dram_in("nw", [128, 256])
    msk_d = P.dram_in("msk", [128, 2, 128])
    z_d = P.dram_out("zT", [128, 2, L], BF16)
    v_s = P.dram_tmp("v_s", [NCH, 128, 256], BF16)
    o_s = P.dram_tmp("o_s", [NCH, 128, 256], BF16)
    y_s = P.dram_tmp("y_s", [NCH, 128, 256], F32)
    g_s = P.dram_tmp("g_s", [4, L], F32)

    R = P.sb([128, 16 * 768 + 16 * 512], BF16)
    w = P.view(R, 0, 16 * 768, pat="p (k n) -> p k n", n=768)
    wg32 = P.sb([128, 16, 4], F32)
    wgb = P.sb([128, 16, 4], BF16)
    cw = P.sb([128, 2, 3], F32)
    bg = P.sb([128, 4], F32)
    nw = P.sb([128, 256], F32)
    msk = P.sb([128, 2, 128], F32)
    qpre = P.sb([128, L], F32)
    kpre = P.sb([128, L], F32)
    qT = P.sb([128, L], BF16)
    kT = P.sb([128, L], BF16)
    hb = P.view(R, 16 * 768, 16 * 768 + 16 * 512, pat="p (k n) -> p k n", n=512)
    vst = P.sb([128, 4, 256], BF16)
    ost = P.sb([128, 4, 256], BF16)
    gst = P.sb([4, 512], F32)
    ident = make_ident_f(P)
    ident_b = P.sb([128, 128], BF16)
    P.copy(ident_b, ident)
    one1 = P.sb([1, 128], F32)
    P.memset(one1, 1.0)
    ps = [P.ps([128, 512], F32) for _ in range(6)]
    psb = [P.ps([128, 1024], BF16) for _ in range(2)]

    P.dma(w[:, 0:8, :], w_d[0:1024, 0:768].re("(k p) n -> p k n", p=128), eng="pool")
    P.dma(w[:, 8:16, :], w_d[1024:2048, 0:768].re("(k p) n -> p k n", p=128), eng="pool")
    P.dma(wg32, wg_d.re("(k p) n -> p k n", p=128))
    P.copy(wgb, wg32)
    for (t_, d_) in ((cw, cw_d), (bg, bg_d), (nw, nw_d), (msk, msk_d)):
        P.dma(t_, d_)

    it = 0
    for bi, (a, b) in enumerate(blocks(L)):
        n = b - a
        nt = n // 128
        P.dma(hb[:, :, 0:n], hT_d[:, :, a:b])
        for j in range(2):
            pq = ps[it % 2]
            it += 1
            for k in range(16):
                P.mm(pq[:, 0:n], w[:, k, j * 128:(j + 1) * 128], hb[:, k, 0:n], start=(k == 0), stop=(k == 15))
            P.copy((qpre if j == 0 else kpre)[:, a:b], pq[:, 0:n], eng="act")
        import os
        SK = os.environ.get('SKIP', '')
        pg = ps[5]
        if 'g' not in SK:
            for k in range(16):
                P.mm(pg[0:4, 0:n], wgb[:, k, :], hb[:, k, 0:n], start=(k == 0), stop=(k == 15))
            P.copy(gst[:, 0:n], pg[0:4, 0:n])
            if 'd' not in SK:
                P.dma(g_s[:, a:b], gst[:, 0:n])
        if 'v' in SK:
            continue
        for ti in range(nt):
            pv = ps[2 + ti % 2]
            for k in range(16):
                P.mm(pv, hb[:, k, ti * 128:(ti + 1) * 128], w[:, k, 256:768], start=(k == 0), stop=(k == 15))
            if 'c' not in SK:
                P.copy(vst[:, ti, :], pv[:, 0:256])
            if 'e' not in SK:
                P.copy(ost[:, ti, :], pv[:, 256:512])
        c0 = a // 128
        if 'o' not in SK:
            P.dma(v_s[c0:c0 + nt].re("c p n -> p c n"), vst[:, 0:nt, :])
            P.dma(o_s[c0:c0 + nt].re("c p n -> p c n"), ost[:, 0:nt, :])

    if "conv" not in stages and "gate" not in stages:
        P.dma(z_d[:, 0, 0:512], hb[:, 0, :], is_output=True)
        P.finish(); return nc
    P.barrier()
    acc = P.view(R, 0, 2 * L, dtype=F32)
    for j, (src, dst) in enumerate(((qpre, qT), (kpre, kT))):
        for (a, b) in ((0, 256), (256, L)):
            P.ts(acc[:, a:b], src[:, a:b], cw[:, j, 1:2], ALU.mult)
            P.stt(acc[:, a + 1:b], src[:, a:b - 1], cw[:, j, 0:1], acc[:, a + 1:b], ALU.mult, ALU.add)
            P.stt(acc[:, a:b - 1], src[:, a + 1:b], cw[:, j, 2:3], acc[:, a:b - 1], ALU.mult, ALU.add)
        for (a, b) in blocks(L, 2112):
            if j == 0:
                P.act(acc[:, a:b], acc[:, a:b], AF.Silu)
                P.ts(dst[:, a:b], acc[:, a:b], 128 ** -0.5, ALU.mult)
            else:
                P.act(dst[:, a:b], acc[:, a:b], AF.Silu)

    NC_ = NCH
    gt = P.sb([NC_, 4, 128], F32)
    for gi in range(4):
        P.dma(gt[:, gi, :], g_s[gi].re("(c n) -> c n", n=128))
    for gi in range(4):
        P.ts(gt[:, gi, :], gt[:, gi, :], bg[0:NC_, gi:gi + 1], ALU.add)
    for d in range(2):
        P.act(gt[:, 2 * d, :], gt[:, 2 * d, :], AF.Tanh, scale=1.0 / 15.0)
        P.ts(gt[:, 2 * d, :], gt[:, 2 * d, :], 15.0, ALU.mult)
        P.act(gt[:, 2 * d + 1, :], gt[:, 2 * d + 1, :], AF.Exp, scale=-1.0)
        P.ts(gt[:, 2 * d + 1, :], gt[:, 2 * d + 1, :], 1.0, ALU.add)
        P.act(gt[:, 2 * d + 1, :], gt[:, 2 * d + 1, :], AF.Ln)
        P.ts(gt[:, 2 * d + 1, :], gt[:, 2 * d + 1, :], -1.0, ALU.mult)

    sc_t = [P.sb([NC_, 128], F32) for _ in range(2)]

    def scan(dst, src, op, rev):
        cur = src
        s = 1
        k = 0
        while s < 128:
            nxt = dst if s == 64 else sc_t[k % 2]
            k += 1
            if not rev:
                P.tt(nxt[:, s:128], cur[:, s:128], cur[:, 0:128 - s], op)
                P.copy(nxt[:, 0:s], cur[:, 0:s])
            else:
                P.tt(nxt[:, 0:128 - s], cur[:, 0:128 - s], cur[:, s:128], op)
                P.copy(nxt[:, 128 - s:128], cur[:, 128 - s:128])
            cur = nxt
            s *= 2

    cols = [[P.sb([128, NC_], F32) for _ in range(5)] for _ in range(2)]
    decbc = [P.sb([128, NC_], F32) for _ in range(2)]
    bb = P.sb([NC_, 128], F32)
    aa = P.sb([NC_, 128], F32)
    ml = P.sb([NC_, 128], F32)
    tmp = P.sb([NC_, 128], F32)
    tb_ = [P.sb([NC_, 128], F32) for _ in range(5)]
    small = P.sb([NC_, 8], F32)
    rows = P.sb([1, 6, NC_ + 2], F32)
    for d in range(2):
        rev = (d == 1)
        last = 0 if rev else 127
        I_, F_ = gt[:, 2 * d, :], gt[:, 2 * d + 1, :]
        scan(bb, F_, ALU.add, rev)
        P.tt(aa, I_, bb, ALU.subtract)
        scan(ml, aa, ALU.max, rev)
        P.copy(small[:, 0:1], bb[:, last:last + 1])
        P.copy(small[:, 2:3], ml[:, last:last + 1])
        P.tt(small[:, 1:2], small[:, 0:1], small[:, 2:3], ALU.add)
        pr = ps[0]
        P.tr(pr[0:1, 0:NC_], small[:, 0:1], ident[0:NC_, 0:NC_])
        P.copy(rows[:, 0, 0:NC_], pr[0:1, 0:NC_])
        P.tr(pr[0:1, 0:NC_], small[:, 1:2], ident[0:NC_, 0:NC_])
        P.copy(rows[:, 1, 0:NC_], pr[0:1, 0:NC_])
        P.memset(rows[:, 5, :], 0.0)
        order = list(range(NC_)) if d == 0 else [1, 0] + list(range(NC_ - 1, 1, -1))
        prev = None
        for c in order:
            sc_ = rows[:, 5, 0:1] if prev is None else rows[:, 2, prev:prev + 1]
            P.copy(rows[:, 3, c:c + 1], sc_)
            P.stt(rows[:, 2, c:c + 1], rows[:, 0, c:c + 1], sc_, rows[:, 1, c:c + 1], ALU.add, ALU.max)
            prev = c
        P.tt(rows[:, 4, 0:NC_], rows[:, 0, 0:NC_], rows[:, 3, 0:NC_], ALU.add)
        P.tt(rows[:, 4, 0:NC_], rows[:, 4, 0:NC_], rows[:, 2, 0:NC_], ALU.subtract)
        P.act(rows[:, 4, 0:NC_], rows[:, 4, 0:NC_], AF.Exp)
        pd = ps[1]
        P.mm(pd[:, 0:NC_], one1, rows[:, 4, 0:NC_])
        P.copy(decbc[d], pd[:, 0:NC_])
        P.mm(pr[0:NC_, 0:1], rows[:, 3, 0:NC_], one1[:, 0:1])
        P.copy(small[:, 3:4], pr[0:NC_, 0:1])
        P.mm(pr[0:NC_, 0:1], rows[:, 2, 0:NC_], one1[:, 0:1])
        P.copy(small[:, 4:5], pr[0:NC_, 0:1])
        P.ts(tmp, ml, small[:, 3:4], ALU.max)
        P.ts(small[:, 6:7], small[:, 2:3], -1.0, ALU.mult)
        P.act(tb_[0], tmp, AF.Exp, scale=-1.0, bias=small[:, 2:3])
        P.act(tb_[1], tmp, AF.Exp, scale=-1.0, bias=small[:, 3:4])
        P.tt(tmp, tmp, bb, ALU.add)
        P.act(tb_[2], tmp, AF.Exp, scale=-1.0)
        P.act(tb_[3], aa, AF.Exp, bias=small[:, 6:7])
        P.tt(small[:, 5:6], small[:, 0:1], small[:, 4:5], ALU.subtract)
        P.act(tb_[4], aa, AF.Exp, bias=small[:, 5:6])
        for ti in range(5):
            pt_ = ps[2 + ti % 2]
            P.tr(pt_[:, 0:NC_], tb_[ti], ident[0:NC_, 0:NC_])
            P.copy(cols[d][ti], pt_[:, 0:NC_])

    if "B" not in stages:
        P.dma(z_d[:, 0, :], qT, is_output=True)
        P.finish(); return nc
    C32 = P.sb([128, 257], F32)
    Cb = [P.sb([128, 257], BF16) for _ in range(2)]
    vch = [P.sb([128, 257], BF16) for _ in range(3)]
    for v in vch:
        P.memset(v[:, 256:257], 1.0)
    och = [P.sb([128, 256], BF16) for _ in range(2)]
    yfb = [P.sb([128, 256], F32) for _ in range(2)]
    sT = [P.sb([128, 128], BF16) for _ in range(2)]
    wk = [P.sb([128, 128], BF16) for _ in range(2)]
    t1 = [P.sb([128, 257], F32) for _ in range(2)]
    num = [P.sb([128, 257], F32) for _ in range(2)]
    st = [P.sb([128, 4], F32) for _ in range(2)]
    yo = [P.sb([128, 256], F32) for _ in range(2)]
    sq = [P.sb([128, 256], F32) for _ in range(2)]
    sgo = [P.sb([128, 256], F32) for _ in range(2)]
    zb = [P.sb([128, 256], BF16) for _ in range(2)]
    zo = [P.sb([128, 2, 128], BF16) for _ in range(2)]
    step = 0
    for d in range(2):
        order = list(range(NC_)) if d == 0 else [1, 0] + list(range(NC_ - 1, 1, -1))
        u_c, in_c, enm_c, ea_c, wk_c = cols[d]
        P.memset(C32, 0.0)
        P.memset(Cb[step % 2], 0.0)
        for c in order:
            a = c * 128
            i2 = step % 2
            cur, nxt = Cb[step % 2], Cb[(step + 1) % 2]
            v = vch[step % 3]
            P.dma(v[:, 0:256], v_s[c])
            pS = ps[0]
            P.mm(pS[:, 0:128], kT[:, a:a + 128], qT[:, a:a + 128])
            P.stt(sT[i2], pS[:, 0:128], ea_c[:, c:c + 1], msk[:, d, :], ALU.mult, ALU.mult)
            pK = psb[0]
            P.tr(pK[:, 0:128], kT[:, a:a + 128], ident_b)
            P.ts(wk[i2], pK[:, 0:128], wk_c[:, c:c + 1], ALU.mult)
            p1 = ps[2]
            p2 = ps[3]
            P.mm(p1[:, 0:257], sT[i2], v)
            P.mm(p2[:, 0:257], qT[:, a:a + 128], cur)
            pC = ps[4]
            P.mm(pC[:, 0:257], wk[i2], v)
            P.stt(C32, C32, decbc[d][:, c:c + 1], pC[:, 0:257], ALU.mult, ALU.add)
            P.copy(nxt, C32, eng="act")
            P.ts(t1[i2], p1[:, 0:257], u_c[:, c:c + 1], ALU.mult)
            P.stt(num[i2], p2[:, 0:257], in_c[:, c:c + 1], t1[i2], ALU.mult, ALU.add)
            s_ = st[i2]
            P.stt(s_[:, 0:1], num[i2][:, 256:257], -1.0, num[i2][:, 256:257], ALU.mult, ALU.max)
            P.ts(s_[:, 0:1], s_[:, 0:1], enm_c[:, c:c + 1], ALU.max)
            P.recip(s_[:, 1:2], s_[:, 0:1])
            y = yo[i2]
            if d == 0:
                P.ts(y, num[i2][:, 0:256], s_[:, 1:2], ALU.mult)
                P.dma(y_s[c], y)
            else:
                yf = yfb[i2]
                o = och[i2]
                P.dma(yf, y_s[c])
                P.dma(o, o_s[c])
                P.stt(y, num[i2][:, 0:256], s_[:, 1:2], yf, ALU.mult, ALU.add)
                P.tt(sq[i2], y, y, ALU.mult, eng="pool")
                P.reduce(s_[:, 2:3], sq[i2], ALU.add)
                P.ts(s_[:, 3:4], s_[:, 2:3], 1.0 / 256, ALU.mult, EPS, ALU.add)
                P.act(s_[:, 3:4], s_[:, 3:4], AF.Sqrt)
                P.recip(s_[:, 3:4], s_[:, 3:4])
                P.stt(sq[i2], y, s_[:, 3:4], nw, ALU.mult, ALU.mult)
                P.act(sgo[i2], o, AF.Sigmoid)
                P.tt(zb[i2], sq[i2], sgo[i2], ALU.mult, eng="pool")
                pT = psb[1]
                for vc in range(2):
                    P.tr(pT[:, vc * 128:(vc + 1) * 128], zb[i2][:, vc * 128:(vc + 1) * 128], ident_b)
                P.copy(zo[i2], pT[:, 0:256].re("p (c n) -> p c n", n=128), eng="act")
                P.dma(z_d[:, :, a:a + 128], zo[i2], is_output=True)
            step += 1
    P.finish()
    return nc


NC = 8
T = 1056

def tok_shard(ctx, lat, r):
    return np.concatenate([ctx[32 * r:32 * r + 32], lat[1024 * r:1024 * r + 1024]], 0)

def to_T(a):
    Tn, Dn = a.shape
    return np.ascontiguousarray(a.T.reshape(Dn // 128, 128, Tn).transpose(1, 0, 2))

def from_T(a):
    p, K, Tn = a.shape
    return np.ascontiguousarray(a.transpose(1, 0, 2).reshape(K * 128, Tn).T)

def pk(v):
    return np.ascontiguousarray(v.reshape(-1, 128).T)

def unshard(parts):
    ctx = np.concatenate([p[:32] for p in parts], 0)
    lat = np.concatenate([p[32:] for p in parts], 0)
    return ctx, lat

def rope_tab(pos, dim, base=10000.0):
    inv = np.power(np.float32(base), -np.arange(0, dim, 2, dtype=np.float32) / np.float32(dim)).astype(np.float32)
    ang = pos.astype(np.float32)[:, None] * inv[None, :]
    return np.cos(ang).astype(np.float32), np.sin(ang).astype(np.float32)

def attn_tables():
    n_ctx, n_lat, GW = 256, 8192, 64
    row = np.repeat(np.arange(n_lat // GW), GW); col = np.tile(np.arange(GW), n_lat // GW)
    cr, sr = rope_tab(row, 64); cc, sc = rope_tab(col, 64)
    cos = np.ones((128, 8448), np.float32); sin = np.zeros((128, 8448), np.float32)
    cos[0:32, 256:] = cr.T; cos[32:64, 256:] = cr.T; cos[64:96, 256:] = cc.T; cos[96:128, 256:] = cc.T
    sin[0:32, 256:] = sr.T; sin[32:64, 256:] = sr.T; sin[64:96, 256:] = sc.T; sin[96:128, 256:] = sc.T
    R = np.zeros((128, 128), np.float32)
    for base in (0, 64):
        for i in range(32):
            R[base + i, base + 32 + i] = -1.0
            R[base + 32 + i, base + i] = 1.0
    return cos, sin, np.ascontiguousarray(R.T)

def ret_tables():
    Ln = 8448
    pos_f = np.arange(Ln)
    s = np.arange(Ln)
    pos_b = np.where(s < 256, 255 - s, 8703 - s)
    cf, sf = rope_tab(pos_f, 256); cb, sb = rope_tab(pos_b, 256)
    tab = np.stack([cf.T, sf.T, cb.T, sb.T], 0).astype(np.float32)
    j = np.arange(128)[:, None].astype(np.float32); i = np.arange(128)[None, :].astype(np.float32)
    E_f = np.maximum(i - j, 0); M_f = (i >= j).astype(np.float32)
    E_b = np.maximum(j - i, 0); M_b = (j >= i).astype(np.float32)
    X_f = np.tile(i + 1, (128, 1)); X_b = np.tile(128 - i, (128, 1))
    cols = np.stack([127 - j[:, 0], j[:, 0], np.full(128, 128.0), np.zeros(128)], 1)
    cst = np.concatenate([E_f, M_f, E_b, M_b, X_f, X_b, cols], 1).astype(np.float32)
    return np.ascontiguousarray(tab), np.ascontiguousarray(cst)


def build_H():
    nc = bass.Bass("TRN2", target_bir_lowering=False)
    P = Prog(nc)
    xT_d = P.dram_in("xT", [128, KD, T])
    vec_d = P.dram_in("vec", [128, 5, KD])
    ho_d = P.dram_out("ho", [128, KD, T], BF16)
    xT = P.sb([128, KD, T], F32)
    hb = P.sb([128, KD, T], BF16)
    vec = P.sb([128, 5, KD], F32)
    gs = P.sb([128, 2, KD], F32)
    ones_f = P.sb([128, 128], F32)
    rstd = P.sb([128, T], F32)
    tmpsq = [P.sb([128, 352], F32) for _ in range(2)]
    tmpT = [P.sb([128, T], F32) for _ in range(2)]
    pss = [P.ps([128, 512], F32) for _ in range(3)]
    for k in range(0, KD, 4):
        P.dma(xT[:, k:k + 4, :], xT_d[:, k:k + 4, :])
    P.dma(vec, vec_d)
    P.memset(ones_f, 1.0)
    for o, sc in ((0, 2), (1, 4)):
        P.ts(gs[:, o, :], vec[:, sc, :], 1.0, ALU.add)
        P.tt(gs[:, o, :], gs[:, o, :], vec[:, 0, :], ALU.mult)
    rms_bc(P, xT, ones_f, pss, rstd, tmpsq)
    modulate_T(P, hb, xT, rstd, gs[:, 0, :], vec[:, 1, :], gs[:, 1, :], vec[:, 3, :], tmpT)
    for k in range(0, KD, 4):
        P.dma(ho_d[:, k:k + 4, :], hb[:, k:k + 4, :], is_output=True)
    P.finish()
    return nc


_PROGS = {}


def _prog(name, fn, *a):
    key = (name,) + a
    if key not in _PROGS:
        _PROGS[key] = fn(*a)
    return _PROGS[key]


def _run(nc, maps):
    return run_bass_kernel_spmd(nc, maps, core_ids=list(range(8))).results


def _seq_hT(hos):
    ctx = np.concatenate([h[:, :, 0:32] for h in hos], 2)
    lat = np.concatenate([h[:, :, 32:] for h in hos], 2)
    return np.ascontiguousarray(np.concatenate([ctx, lat], 2))


def _shard_zT(zT_all, r):
    return np.ascontiguousarray(np.concatenate([zT_all[:, :, 32 * r:32 * r + 32], zT_all[:, :, 256 + 1024 * r:256 + 1024 * (r + 1)]], 2))


def kernel(x, c, ctx, c_ctx, w_ada, b_ada, norm_g, ret_w_in, ret_logit_gamma, ret_gn_w, ret_gn_b, ret_w_out,
           mlstm_w_in, mlstm_conv_w, mlstm_b_gate, mlstm_norm_w, mlstm_w_out, attn_w_in, attn_q_norm,
           attn_k_norm, attn_w_out, moe_w_router_group, moe_b_router_group, moe_w_router_expert,
           moe_b_router_expert, moe_w_gate, moe_w_up, moe_w_down, final_norm_g):
    f32 = np.float32
    A = lambda a: np.ascontiguousarray(np.asarray(a, dtype=f32))
    x, c, ctx, c_ctx = A(x), A(c), A(ctx), A(c_ctx)
    w_ada, b_ada, norm_g = A(w_ada), A(b_ada), A(norm_g)
    cs = np.stack([pk(c[0]), pk(c_ctx)], 2).astype(f32)
    maps = []
    for r in range(8):
        maps.append({"cs": cs, "w": np.ascontiguousarray(w_ada[:, :, r * 1536:(r + 1) * 1536]),
                     "b": np.ascontiguousarray(np.tile(b_ada[None, :, r * 1536:(r + 1) * 1536], (2, 1, 1)))})
    res = _run(_prog("ada", build_ADA), maps)
    mod = np.concatenate([res[r]["mod"] for r in range(8)], 2)
    mv = lambda j, i, m: pk(mod[j, i, m * 2048:(m + 1) * 2048])
    xT = [to_T(tok_shard(ctx[0], x[0], r)) for r in range(8)]
    vec = np.ascontiguousarray(np.stack([pk(norm_g[0, 0]), mv(0, 0, 0), mv(0, 0, 1), mv(1, 0, 0), mv(1, 0, 1)], 1).astype(f32))
    res = _run(_prog("h", build_H), [{"xT": xT[r], "vec": vec} for r in range(8)])
    hos = [res[r]["ho"] for r in range(8)]
    out = None
    for i in range(4):
        kind, j = i % 3, i // 3
        hT = _seq_hT(hos)
        if kind == 0:
            tab, cst = ret_tables()
            W = A(ret_w_in[j])
            maps = []
            for hd in range(8):
                w = np.concatenate([W[:, hd * 256:(hd + 1) * 256], W[:, 2048 + hd * 256:2048 + (hd + 1) * 256],
                                    W[:, 4096 + hd * 512:4096 + (hd + 1) * 512], W[:, 8192 + hd * 512:8192 + (hd + 1) * 512]], 1)
                lgam = np.tile(A(ret_logit_gamma[j])[:, hd][None, :], (128, 1)).astype(f32)
                gn = np.stack([np.tile(A(ret_gn_w[j])[hd * 512:(hd + 1) * 512][None], (128, 1)),
                               np.tile(A(ret_gn_b[j])[hd * 512:(hd + 1) * 512][None], (128, 1))], 1).astype(f32)
                maps.append({"hT": hT, "w": np.ascontiguousarray(w), "lgam": lgam, "gn": np.ascontiguousarray(gn), "tab": tab, "cst": cst})
            res = _run(_prog("ret", build_RET), maps)
            KZ, w_out = 32, A(ret_w_out[j])
        elif kind == 1:
            W = A(mlstm_w_in[j])
            jj = np.arange(128)[:, None]; ii = np.arange(128)[None, :]
            msk = np.ascontiguousarray(np.stack([(ii >= jj), (jj >= ii)], 1).astype(f32))
            maps = []
            for hd in range(8):
                gcols = [6144 + dd * 16 + gg * 8 + hd for dd in range(2) for gg in range(2)]
                w = np.concatenate([W[:, hd * 128:(hd + 1) * 128], W[:, 1024 + hd * 128:1024 + (hd + 1) * 128],
                                    W[:, 2048 + hd * 256:2048 + (hd + 1) * 256], W[:, 4096 + hd * 256:4096 + (hd + 1) * 256]], 1)
                wg = np.ascontiguousarray(W[:, gcols])
                cwv = A(mlstm_conv_w[j])
                cw = np.ascontiguousarray(np.stack([cwv[:, hd * 128:(hd + 1) * 128].T, cwv[:, 1024 + hd * 128:1024 + (hd + 1) * 128].T], 1).astype(f32))
                bgv = A(mlstm_b_gate[j])
                bg = np.tile(np.array([bgv[dd, gg, hd] for dd in range(2) for gg in range(2)], f32)[None], (128, 1))
                nw = np.tile(A(mlstm_norm_w[j])[hd * 256:(hd + 1) * 256][None], (128, 1)).astype(f32)
                maps.append({"hT": hT, "wg": wg, "w": np.ascontiguousarray(w), "cw": cw, "bg": bg, "nw": nw, "msk": msk})
            res = _run(_prog("mls", build_MLS), maps)
            KZ, w_out = 16, A(mlstm_w_out[j])
        else:
            cos, sin, RT = attn_tables()
            W = A(attn_w_in[j])
            nw = np.ascontiguousarray(np.stack([A(attn_q_norm[j]), A(attn_k_norm[j])], 1).astype(f32))
            maps = []
            for g in range(8):
                w = np.concatenate([W[:, (2 * g) * 128:(2 * g + 2) * 128], W[:, 2048 + g * 128:2048 + (g + 1) * 128],
                                    W[:, 3072 + g * 128:3072 + (g + 1) * 128]], 1)
                maps.append({"hT": hT, "w": np.ascontiguousarray(w), "nw": nw, "cosT": cos, "sinT": sin, "RT": RT})
            res = _run(_prog("att", build_ATT), maps)
            KZ, w_out = 16, A(attn_w_out[j])
        zT_all = np.concatenate([res[g]["zT"] for g in range(8)], 1)
        gate = np.ascontiguousarray(np.stack([mv(0, i, 2), mv(1, i, 2)], 1).astype(f32))
        res = _run(_prog("p", build_P, KZ), [{"xT": xT[r], "zT": _shard_zT(zT_all, r), "w": w_out, "gate": gate} for r in range(8)])
        xT = [res[r]["xo"] for r in range(8)]
        final = (i == 3)
        if final:
            nxt = [pk(A(final_norm_g))] + [np.zeros((128, 16), f32)] * 4
        else:
            nxt = [pk(norm_g[i + 1, 0]), mv(0, i + 1, 0), mv(0, i + 1, 1), mv(1, i + 1, 0), mv(1, i + 1, 1)]
        vec = np.ascontiguousarray(np.stack([pk(norm_g[i, 1]), mv(0, i, 3), mv(0, i, 4), mv(0, i, 5), mv(1, i, 3), mv(1, i, 4), mv(1, i, 5)] + nxt, 1).astype(f32))
        wr0 = np.concatenate([A(moe_w_router_group[i]), A(moe_w_router_expert[i])], 1)
        wr = np.ascontiguousarray(wr0.reshape(16, 128, 36).transpose(1, 0, 2))
        br = np.ascontiguousarray(np.tile(np.concatenate([A(moe_b_router_group[i]), A(moe_b_router_expert[i])])[None], (128, 1)).astype(f32))
        wg_, wu_, wd_ = A(moe_w_gate[i]), A(moe_w_up[i]), A(moe_w_down[i])
        res = _run(_prog("f", build_F, final), [{"xT": xT[r], "vec": vec, "wr": wr, "br": br, "wg": wg_, "wu": wu_, "wd": wd_} for r in range(8)])
        xT = [res[r]["xo"] for r in range(8)]
        hos = [res[r]["ho"] for r in range(8)]
    lat = np.concatenate([from_T(np.asarray(h, dtype=f32))[32:] for h in hos], 0)
    return np.ascontiguousarray(lat.reshape(1, 8192, 2048).astype(f32))
```
